# Optimizing a Trainium2 kernel written in Bass

```python
import jax, jax.numpy as jnp
from jax import lax
import numpy as np

D_MODEL = 1024
BATCH = 4
SEQ = 8192
DEPTH = 1

HEAD_DIM = 64
SWA_Q_HEADS = 6
SWA_KV_HEADS = 2
SWA_WINDOW = 128
SWA_BLOCK = 128
MOBA_HEADS = 6
MOBA_BLOCK = 256
MOBA_TOPK = 3
MOBA_Q_CHUNK = 64
MEM_HEADS = 4
MEM_LEN = 256
N_BRANCH = 3
D_FF = 2816
LN_EPS = 1e-5
DN_ALPHA = (2 * DEPTH) ** 0.25
DN_BETA = (8 * DEPTH) ** -0.25

SWA_Q_W = SWA_Q_HEADS * HEAD_DIM
SWA_KV_W = SWA_KV_HEADS * HEAD_DIM
MOBA_W = MOBA_HEADS * HEAD_DIM
MEM_W = MEM_HEADS * HEAD_DIM
GATE_W = N_BRANCH * D_MODEL
IN_W = SWA_Q_W + 2 * SWA_KV_W + 3 * MOBA_W + MEM_W + GATE_W

kernel_name = 'hybrid_swa_moba_mem_macaron_deepnorm'


def layer_norm(x, g, b):
    xf = x.astype(jnp.float32)
    mu = jnp.mean(xf, axis=-1, keepdims=True)
    var = jnp.mean(jnp.square(xf - mu), axis=-1, keepdims=True)
    return ((xf - mu) * lax.rsqrt(var + LN_EPS) * g.astype(jnp.float32) + b.astype(jnp.float32)).astype(x.dtype)


def swiglu(x, w_gate, w_up, w_down):
    return (jax.nn.silu(x @ w_gate) * (x @ w_up)) @ w_down


def alibi_slopes(n):
    return jnp.exp2(-8.0 * jnp.arange(1, n + 1, dtype=jnp.float32) / n)


def sliding_window_gqa(q, k, v, sinks):
    B, S, _ = q.shape
    nb = S // SWA_BLOCK
    G = SWA_Q_HEADS // SWA_KV_HEADS
    qb = q.reshape(B, nb, SWA_BLOCK, SWA_KV_HEADS, G, HEAD_DIM)
    kb = k.reshape(B, nb, SWA_BLOCK, SWA_KV_HEADS, HEAD_DIM)
    vb = v.reshape(B, nb, SWA_BLOCK, SWA_KV_HEADS, HEAD_DIM)

    def with_prev(xb):
        prev = jnp.pad(xb[:, :-1], ((0, 0), (1, 0), (0, 0), (0, 0), (0, 0)))
        return jnp.concatenate([prev, xb], axis=2)

    kc, vc = with_prev(kb), with_prev(vb)
    s = jnp.einsum('bnqkgd,bnskd->bkgnqs', qb, kc).astype(jnp.float32) * (HEAD_DIM ** -0.5)
    blk = jnp.arange(nb)[:, None, None] * SWA_BLOCK
    qpos = blk + jnp.arange(SWA_BLOCK)[None, :, None]
    kpos = blk - SWA_BLOCK + jnp.arange(2 * SWA_BLOCK)[None, None, :]
    dist = qpos - kpos
    mask = (dist >= 0) & (dist < SWA_WINDOW) & (kpos >= 0)
    slopes = alibi_slopes(SWA_Q_HEADS).reshape(SWA_KV_HEADS, G)
    s = s - slopes[:, :, None, None, None] * dist.astype(jnp.float32)
    s = jnp.where(mask, s, -jnp.inf)
    sink = jnp.broadcast_to(sinks.astype(jnp.float32).reshape(SWA_KV_HEADS, G)[None, :, :, None, None, None], s.shape[:-1] + (1,))
    p = jax.nn.softmax(jnp.concatenate([s, sink], axis=-1), axis=-1)[..., :-1]
    o = jnp.einsum('bkgnqs,bnskd->bnqkgd', p.astype(v.dtype), vc)
    return o.reshape(B, S, SWA_Q_W)


def moba_attention(q, k, v):
    B, S, _ = q.shape
    H = MOBA_HEADS
    S_pad = -(-S // MOBA_BLOCK) * MOBA_BLOCK
    nblk = S_pad // MOBA_BLOCK
    scale = HEAD_DIM ** -0.5

    def heads(t):
        t = t.reshape(B, S, H, HEAD_DIM).transpose(0, 2, 1, 3)
        return jnp.pad(t, ((0, 0), (0, 0), (0, S_pad - S), (0, 0)))

    qh, kh, vh = heads(q), heads(k), heads(v)
    kblk = kh.reshape(B, H, nblk, MOBA_BLOCK, HEAD_DIM)
    vblk = vh.reshape(B, H, nblk, MOBA_BLOCK, HEAD_DIM)
    k_mean = jnp.mean(kblk.astype(jnp.float32), axis=3)
    gate = jnp.einsum('bhtd,bhnd->bhtn', qh.astype(jnp.float32), k_mean)
    qblk = jnp.arange(S_pad) // MOBA_BLOCK
    fully_past = jnp.arange(nblk)[None, :] < qblk[:, None]
    gate = jnp.where(fully_past, gate, -jnp.inf)
    n_sel = min(MOBA_TOPK, nblk)
    _, sel = lax.top_k(gate, n_sel)
    slopes = alibi_slopes(H)[None, :, None, None]
    bi = jnp.arange(B)[:, None, None, None]
    hi = jnp.arange(H)[None, :, None, None]

    def chunk(c):
        t0 = c * MOBA_Q_CHUNK
        qc = lax.dynamic_slice_in_dim(qh, t0, MOBA_Q_CHUNK, axis=2)
        sc = lax.dynamic_slice_in_dim(sel, t0, MOBA_Q_CHUNK, axis=2)
        tq = t0 + jnp.arange(MOBA_Q_CHUNK)
        own = t0 // MOBA_BLOCK
        k_sel = kblk[bi, hi, sc]
        v_sel = vblk[bi, hi, sc]
        s_sel = jnp.einsum('bhqd,bhqjsd->bhqjs', qc, k_sel).astype(jnp.float32) * scale
        kpos_sel = sc[..., None] * MOBA_BLOCK + jnp.arange(MOBA_BLOCK)
        s_sel = s_sel - slopes[..., None] * (tq[:, None, None] - kpos_sel).astype(jnp.float32)
        s_sel = jnp.where((sc < own)[..., None], s_sel, -jnp.inf)
        s_sel = s_sel.reshape(B, H, MOBA_Q_CHUNK, n_sel * MOBA_BLOCK)
        k_own = lax.dynamic_index_in_dim(kblk, own, axis=2, keepdims=False)
        v_own = lax.dynamic_index_in_dim(vblk, own, axis=2, keepdims=False)
        s_own = jnp.einsum('bhqd,bhsd->bhqs', qc, k_own).astype(jnp.float32) * scale
        dist_own = tq[:, None] - (own * MOBA_BLOCK + jnp.arange(MOBA_BLOCK))[None, :]
        s_own = s_own - slopes * dist_own.astype(jnp.float32)
        s_own = jnp.where(dist_own >= 0, s_own, -jnp.inf)
        p = jax.nn.softmax(jnp.concatenate([s_sel, s_own], axis=-1), axis=-1).astype(v.dtype)
        p_sel = p[..., :n_sel * MOBA_BLOCK].reshape(B, H, MOBA_Q_CHUNK, n_sel, MOBA_BLOCK)
        p_own = p[..., n_sel * MOBA_BLOCK:]
        return (jnp.einsum('bhqjs,bhqjsd->bhqd', p_sel, v_sel)
                + jnp.einsum('bhqs,bhsd->bhqd', p_own, v_own))

    o = lax.map(chunk, jnp.arange(S_pad // MOBA_Q_CHUNK))
    o = o.transpose(1, 2, 0, 3, 4).reshape(B, H, S_pad, HEAD_DIM)[:, :, :S]
    return o.transpose(0, 2, 1, 3).reshape(B, S, MOBA_W)


def memory_cross_attention(q, mem, w_mem_kv, b_mem_kv):
    B, S, _ = q.shape
    M = mem.shape[1]
    kv = mem @ w_mem_kv + b_mem_kv
    k, v = jnp.split(kv, 2, axis=-1)
    qh = q.reshape(B, S, MEM_HEADS, HEAD_DIM)
    kh = k.reshape(B, M, MEM_HEADS, HEAD_DIM)
    vh = v.reshape(B, M, MEM_HEADS, HEAD_DIM)
    s = jnp.einsum('bthd,bmhd->bhtm', qh, kh).astype(jnp.float32) * (HEAD_DIM ** -0.5)
    p = jax.nn.softmax(s, axis=-1).astype(v.dtype)
    return jnp.einsum('bhtm,bmhd->bthd', p, vh).reshape(B, S, MEM_W)


def hybrid_layer(x, mem, ffn1_w_gate, ffn1_w_up, ffn1_w_down, ln1_g, ln1_b, w_in, b_in,
                 attn_sinks, w_mem_kv, b_mem_kv, w_proj_swa, w_proj_moba, w_proj_mem, w_out,
                 ln2_g, ln2_b, ffn2_w_gate, ffn2_w_up, ffn2_w_down, ln3_g, ln3_b):
    B, S, D = x.shape
    x = layer_norm(DN_ALPHA * x + 0.5 * swiglu(x, ffn1_w_gate, ffn1_w_up, ffn1_w_down), ln1_g, ln1_b)
    h = x @ w_in + b_in
    splits = [SWA_Q_W, SWA_KV_W, SWA_KV_W, MOBA_W, MOBA_W, MOBA_W, MEM_W]
    offsets = [int(o) for o in np.cumsum(splits)]
    q_a, k_a, v_a, q_b, k_b, v_b, q_m, gates = jnp.split(h, offsets, axis=-1)
    y_a = sliding_window_gqa(q_a, k_a, v_a, attn_sinks) @ w_proj_swa
    y_b = moba_attention(q_b, k_b, v_b) @ w_proj_moba
    y_m = memory_cross_attention(q_m, mem, w_mem_kv, b_mem_kv) @ w_proj_mem
    g = jax.nn.sigmoid(gates.reshape(B, S, N_BRANCH, D))
    mixed = (g[:, :, 0] * y_a + g[:, :, 1] * y_b + g[:, :, 2] * y_m) @ w_out
    x = layer_norm(DN_ALPHA * x + mixed, ln2_g, ln2_b)
    x = layer_norm(DN_ALPHA * x + 0.5 * swiglu(x, ffn2_w_gate, ffn2_w_up, ffn2_w_down), ln3_g, ln3_b)
    return x


def setup_inputs(seed: int = 0) -> dict:
    key = jax.random.key(seed)
    ks = jax.random.split(key, 24)
    f32 = jnp.float32

    def w(k, shape, fan_in, gain=1.0):
        return jax.random.normal(k, (DEPTH,) + shape, f32) * (gain * fan_in ** -0.5)

    def small(k, shape, s=0.02):
        return jax.random.normal(k, (DEPTH,) + shape, f32) * s

    return {
        'x': jax.random.normal(ks[0], (BATCH, SEQ, D_MODEL), f32),
        'mem': jax.random.normal(ks[1], (BATCH, MEM_LEN, D_MODEL), f32),
        'ffn1_w_gate': w(ks[2], (D_MODEL, D_FF), D_MODEL),
        'ffn1_w_up': w(ks[3], (D_MODEL, D_FF), D_MODEL),
        'ffn1_w_down': w(ks[4], (D_FF, D_MODEL), D_FF, DN_BETA),
        'ln1_g': 1.0 + small(ks[5], (D_MODEL,)),
        'ln1_b': small(ks[6], (D_MODEL,)),
        'w_in': w(ks[7], (D_MODEL, IN_W), D_MODEL),
        'b_in': small(ks[8], (IN_W,)),
        'attn_sinks': small(ks[9], (SWA_Q_HEADS,), 0.5),
        'w_mem_kv': w(ks[10], (D_MODEL, 2 * MEM_W), D_MODEL),
        'b_mem_kv': small(ks[11], (2 * MEM_W,)),
        'w_proj_swa': w(ks[12], (SWA_Q_W, D_MODEL), SWA_Q_W, DN_BETA),
        'w_proj_moba': w(ks[13], (MOBA_W, D_MODEL), MOBA_W, DN_BETA),
        'w_proj_mem': w(ks[14], (MEM_W, D_MODEL), MEM_W, DN_BETA),
        'w_out': w(ks[15], (D_MODEL, D_MODEL), D_MODEL, DN_BETA),
        'ln2_g': 1.0 + small(ks[16], (D_MODEL,)),
        'ln2_b': small(ks[17], (D_MODEL,)),
        'ffn2_w_gate': w(ks[18], (D_MODEL, D_FF), D_MODEL),
        'ffn2_w_up': w(ks[19], (D_MODEL, D_FF), D_MODEL),
        'ffn2_w_down': w(ks[20], (D_FF, D_MODEL), D_FF, DN_BETA),
        'ln3_g': 1.0 + small(ks[21], (D_MODEL,)),
        'ln3_b': small(ks[22], (D_MODEL,)),
    }


def reference(x, mem, ffn1_w_gate, ffn1_w_up, ffn1_w_down, ln1_g, ln1_b, w_in, b_in,
              attn_sinks, w_mem_kv, b_mem_kv, w_proj_swa, w_proj_moba, w_proj_mem, w_out,
              ln2_g, ln2_b, ffn2_w_gate, ffn2_w_up, ffn2_w_down, ln3_g, ln3_b):
    for l in range(DEPTH):
        x = hybrid_layer(x, mem, ffn1_w_gate[l], ffn1_w_up[l], ffn1_w_down[l], ln1_g[l], ln1_b[l],
                         w_in[l], b_in[l], attn_sinks[l], w_mem_kv[l], b_mem_kv[l],
                         w_proj_swa[l], w_proj_moba[l], w_proj_mem[l], w_out[l],
                         ln2_g[l], ln2_b[l], ffn2_w_gate[l], ffn2_w_up[l], ffn2_w_down[l],
                         ln3_g[l], ln3_b[l])
    return x
```

```python
import contextlib
import numpy as np
import ml_dtypes
import concourse.bass as bass
import concourse.mybir as mybir
from concourse.bass_utils import run_bass_kernel_spmd

F32 = mybir.dt.float32
BF16 = mybir.dt.bfloat16
AF = mybir.ActivationFunctionType
ALU = mybir.AluOpType
AX = mybir.AxisListType

ALPHA = float(2.0 ** 0.25)
EPS = 1e-5
NT = 8
T = 512
NSLOT = 4
SELF_SYNC = True
SLOPES = [2.0 ** (-8.0 * h / 6.0) for h in range(1, 7)]

U_FFN = {1: 0, 2: 19}
U_WA, U_WB, U_WC, U_WV = 38, 39, 40, 41
U_MKV = 42
U_MIX = 43
U_WOUT = 51
NU = 53

_cf_off = {}
_cf_n = 0
def _cf(name, n):
    global _cf_n
    _cf_off[name] = _cf_n
    _cf_n += n
for _n, _w in [("ident", 128), ("ones", 128), ("eps", 1), ("g1", 8), ("b1", 8), ("g2", 8), ("b2", 8),
               ("binfm", 36), ("bmkfm", 2), ("sinks", 6), ("swacur", 6), ("swaprev", 6), ("sinkc", 6),
               ("mb0", 6), ("mb1", 6), ("tg", 2 * 2 * 6 * 46), ("vbias", 16 * 32), ("cmask", 2),
               ("binv", 512), ("bmkv", 256), ("g3", 1024), ("b3", 1024)]:
    _cf(_n, _w)
NCF = _cf_n
NCB = 384


class _Op:
    __slots__ = ("eng", "fn", "deps", "signal", "sigval", "dma", "dma_sem", "dma_val", "dma_prev")


class Sched:
    ENG = ["pe", "act", "dve", "pool", "sp"]

    def __init__(self):
        self.ops = []
        self.eng_ops = {e: [] for e in self.ENG}
        self.lastw = {}
        self.readers = {}
        self.dma_cls = {}

    def add_dma_class(self, name, sems):
        self.dma_cls[name] = {"ring": sems, "count": 0}

    def add(self, eng, fn, r=(), w=(), dma=None):
        op = _Op()
        op.eng, op.fn, op.dma, op.signal, op.sigval = eng, fn, dma, False, 0
        deps = {}
        for k in r:
            p = self.lastw.get(k)
            if p is not None:
                deps[id(p)] = p
            if k[0] == "ps":
                rd = self.readers.get(k)
                if rd:
                    for e2, q in rd[0].items():
                        if e2 != eng:
                            deps[id(q)] = q
        for k in w:
            p = self.lastw.get(k)
            if p is not None:
                deps[id(p)] = p
            rd = self.readers.get(k)
            if rd:
                for q in rd[0].values():
                    deps[id(q)] = q
                for q in rd[1]:
                    deps[id(q)] = q
        for k in r:
            rd = self.readers.get(k)
            if rd is None:
                rd = self.readers[k] = [{}, []]
            if dma is None:
                rd[0][eng] = op
            else:
                rd[1].append(op)
        for k in w:
            self.lastw[k] = op
            self.readers[k] = [{}, []]
        op.deps = []
        for p in deps.values():
            if p is op:
                continue
            if p.dma is None and p.eng == eng and dma is None:
                if eng == "pe" or not SELF_SYNC:
                    continue
            op.deps.append(p)
            if p.dma is None:
                p.signal = True
        if dma is not None:
            cls = self.dma_cls[dma]
            i = cls["count"]
            cls["count"] += 1
            R = len(cls["ring"])
            op.dma_sem = cls["ring"][i % R]
            op.dma_val = 16 * (i // R + 1)
            op.dma_prev = 16 * (i // R)
        self.ops.append(op)
        self.eng_ops[eng].append(op)
        return op

    def emit(self, nc, block, esem):
        cnt = {e: 0 for e in self.ENG}
        for op in self.ops:
            if op.dma is None and op.signal:
                cnt[op.eng] += 1
                op.sigval = cnt[op.eng]
        blk = {"pe": block.tensor, "act": block.scalar, "dve": block.vector, "pool": block.gpsimd, "sp": block.sync}
        for eng in self.ENG:
            ops = self.eng_ops[eng]

            def body(e, ops=ops, eng=eng):
                waited = {}
                for op in ops:
                    need = {}
                    for p in op.deps:
                        if p.dma is not None:
                            sem, v = p.dma_sem, p.dma_val
                        else:
                            sem, v = esem[p.eng], p.sigval
                        k = id(sem)
                        if k not in need or need[k][1] < v:
                            need[k] = (sem, v)
                    if op.dma is not None and op.dma_prev > 0:
                        k = id(op.dma_sem)
                        if k not in need or need[k][1] < op.dma_prev:
                            need[k] = (op.dma_sem, op.dma_prev)
                    for k, (sem, v) in need.items():
                        if waited.get(k, 0) < v:
                            e.wait_ge(sem, v)
                            waited[k] = v
                    if op.fn is None:
                        continue
                    ins = op.fn(e)
                    if op.dma is not None:
                        ins.then_inc(op.dma_sem, 16)
                    elif op.signal:
                        ins.then_inc(esem[eng], 1)

            blk[eng](body)


def build_nc(n_other=NT, n_main=NT, skip_first=False, dbg=None, upto=99):
    nc = bass.Bass("TRN2", target_bir_lowering=False)

    def din(name, shape, dt=F32):
        return nc.dram_tensor(name, list(shape), dt, kind="ExternalInput").ap()

    x_main = din("x_main", [4096, 1024])
    x_other = din("x_other", [4096, 1024])
    mem = din("mem", [256, 1024])
    Wd = {}
    for k in (1, 2):
        Wd[f"wg{k}"] = din(f"wg{k}", [1024, 2816])
        Wd[f"wu{k}"] = din(f"wu{k}", [1024, 2816])
        Wd[f"wd{k}"] = din(f"wd{k}", [2816, 1024])
    w_in = din("w_in", [1024, 5120])
    w_mkv = din("w_mkv", [1024, 512])
    w_pswa = din("w_pswa", [384, 1024])
    w_pmoba = din("w_pmoba", [384, 1024])
    w_pmem = din("w_pmem", [256, 1024])
    w_out = din("w_out", [1024, 1024])
    cf_d = din("cf", [128, NCF])
    cb_d = din("cb", [128, NCB], BF16)
    out = nc.dram_tensor("out", [4096, 1024], F32, kind="ExternalOutput").ap()
    dbg_d = None
    if dbg is not None:
        dbg_d = nc.dram_tensor("dbg", [128, dbg[1]], F32, kind="ExternalOutput").ap()
    WS = nc.dram_tensor("WS", [NU, 128, 4096], BF16).ap()
    KTs = nc.dram_tensor("KTs", [3, 128, 8192], BF16).ap()
    Vs = nc.dram_tensor("Vs", [8192, 390], BF16).ap()

    S = Sched()
    with contextlib.ExitStack() as es:
        def sb(name, shape, dt):
            return es.enter_context(nc.sbuf_tensor("sb_" + name, list(shape), dt))

        def sem(name):
            return es.enter_context(nc.semaphore(name))

        cf = sb("cf", [128, NCF], F32)
        cb = sb("cb", [128, NCB], BF16)
        prm = sb("prm", [128, 64], F32)
        xa = sb("xa", [128, 8, T], F32)
        xb = sb("xb", [128, 8, T], BF16)
        hT = sb("hT", [128, 22, T], BF16)
        sg = [sb(f"sg{i}", [128, T], F32) for i in range(2)]
        sq = sg
        st = sb("st", [1, 4, T], F32)
        wsl = [sb(f"wsl{i}", [128, 4096], BF16) for i in range(NSLOT)]
        QA = sb("QA", [128, 3, T], BF16)
        QB = sb("QB", [128, 3, T], BF16)
        QM = sb("QM", [128, 2, T], BF16)
        KBc = sb("KBc", [128, 3, T], BF16)
        VBc = sb("VBc", [128, 4, 6, 65], BF16)
        KA = sb("KA", [128, 128 + 4096], BF16)
        VA = sb("VA", [128, 33, 2, 65], BF16)
        KM = sb("KM", [128, 2, 256], BF16)
        VM = sb("VM", [128, 2, 4, 65], BF16)
        memT = sb("memT", [128, 8, 256], BF16)
        KmT = sb("KmT", [128, 3, 32], BF16)
        KS = [sb(f"KS{i}", [128, 3, 256], BF16) for i in range(3)]
        VS = [sb(f"VS{i}", [128, 2, 6, 65], BF16) for i in range(3)]
        NPT = 6
        PT = [sb(f"PT{i}", [128, T], BF16) for i in range(NPT)]
        acc = sb("acc", [128, 4, 6, 65], F32)
        gb = sb("gb", [128, 4, 6, 32], F32)
        fT = sb("fT", [128, 4, 6, 32], F32)
        t8 = sb("t8", [128, 24, 8], F32)
        thr = sb("thr", [128, 24], F32)
        den = sb("den", [128, 24], F32)
        Otok = sb("Otok", [128, 4, 1024], F32)
        xs = [Otok[:, 2, :], Otok[:, 3, :]]
        ostg = [Otok[:, 0, :], Otok[:, 1, :]]
        OT = hT[:, 0:8, :]
        mixed = hT[:, 8:16, :]
        _mtv = hT[:, 16:22, :].rearrange("p a b -> p (a b)").bitcast(F32).rearrange("p (r n) -> p r n", n=T)
        mt = [_mtv[:, i, :] for i in range(3)]
        R_OT = [("hT", i) for i in range(8)]
        R_MIXED = [("hT", i) for i in range(8, 16)]
        R_MT = [[("hT", 16 + 2 * i), ("hT", 17 + 2 * i)] for i in range(3)]
        lst = sb("lst", [128, 2, 6], F32)
        lmv = sb("lmv", [128, 4], F32)
        PS = [es.enter_context(nc.psum_tensor(f"ps{i}", [128, 512], F32)) for i in range(8)]

        esem = {e: sem(f"e_{e}") for e in ("pe", "act", "dve", "pool", "sp")}
        S.add_dma_class("init", [sem("d_init")])
        S.add_dma_class("cast", [sem(f"d_cast{i}") for i in range(8)])
        S.add_dma_class("w", [sem(f"d_w{i}") for i in range(NSLOT)])
        S.add_dma_class("xl", [sem(f"d_xl{i}") for i in range(2)])
        S.add_dma_class("kvl", [sem(f"d_kvl{i}") for i in range(6)])
        S.add_dma_class("kvs", [sem(f"d_kvs{i}") for i in range(4)])
        S.add_dma_class("out", [sem(f"d_out{i}") for i in range(2)])

        C = _cf_off

        def cfc(name, i=0, n=1):
            return cf[:, C[name] + i:C[name] + i + n]

        ident = cf[:, C["ident"]:C["ident"] + 128]
        ones_col = cf[:, C["ones"]:C["ones"] + 1]
        ones_row = cf[0:1, C["ones"]:C["ones"] + 128]
        eps_row = cf[0:1, C["eps"]:C["eps"] + 1]
        eps_col = cf[:, C["eps"]:C["eps"] + 1]
        identb = cb[:, 0:128]
        causal = cb[:, 128:256]
        anti = cb[:, 256:384]

        pscnt = [0]

        def newps():
            b = pscnt[0] % 8
            pscnt[0] += 1
            return b

        def MM(o, l, rr, start, stop, r, w):
            S.add("pe", lambda e: e.matmul(o, l, rr, start=start, stop=stop), r=r, w=w)

        def TR(o, i, r, w):
            S.add("pe", lambda e: e.transpose(o, i, ident), r=r, w=w)

        def ACTF(o, i, func, r, w, bias=None, scale=None):
            kw = {}
            if bias is not None:
                kw["bias"] = bias
            if scale is not None:
                kw["scale"] = scale
            S.add("act", lambda e: e.activation(out=o, in_=i, func=func, **kw), r=r, w=w)

        def TT(eng, o, a, b, op, r, w):
            S.add(eng, lambda e: e.tensor_tensor(out=o, in0=a, in1=b, op=op), r=r, w=w)

        def TS(eng, o, a, s1, s2, op0, op1, r, w):
            if op1 is None:
                S.add(eng, lambda e: e.tensor_scalar(out=o, in0=a, scalar1=s1, scalar2=None, op0=op0), r=r, w=w)
            else:
                S.add(eng, lambda e: e.tensor_scalar(out=o, in0=a, scalar1=s1, scalar2=s2, op0=op0, op1=op1), r=r, w=w)

        def STT(o, a, sc, b, op0, op1, r, w):
            S.add("dve", lambda e: e.scalar_tensor_tensor(out=o, in0=a, scalar=sc, in1=b, op0=op0, op1=op1), r=r, w=w)

        def CP(eng, o, i, r, w):
            S.add(eng, lambda e: e.tensor_copy(out=o, in_=i), r=r, w=w)

        def MEMSET(eng, o, v, r, w):
            S.add(eng, lambda e: e.memset(o, v), r=r, w=w)

        def DMA(eng, cls, o, i, r, w):
            S.add(eng, lambda e: e.dma_start(out=o, in_=i), r=r, w=w, dma=cls)

        ws_parts = {}

        def wspart(u):
            lst_ = ws_parts.setdefault(u, [])
            key = ("ws", u, len(lst_))
            lst_.append(key)
            return key

        def cast(u, off, n_inner, src, pattern, **kw):
            srcv = src.rearrange(pattern, **kw)
            shp = list(srcv.shape)
            tot = 1
            for d in shp[1:]:
                tot *= d
            dst = WS[u][:, off:off + tot]
            if len(shp) == 3:
                dst = dst.rearrange("p (a b) -> p a b", b=shp[2])
            DMA("pool", "cast", dst, srcv, r=[], w=[wspart(u)])

        def cast_ffn(k):
            base = U_FFN[k]
            for j in range(11):
                for fcl in range(2):
                    fc = 2 * j + fcl
                    for gu, nm in ((0, f"wg{k}"), (1, f"wu{k}")):
                        cast(base + j, ((fcl * 2 + gu) * 8) * 128, 128, Wd[nm][:, fc * 128:(fc + 1) * 128],
                             "(kc p) n -> p kc n", p=128)
            for c in range(8):
                cast(base + 11 + c, 0, 128, Wd[f"wd{k}"][:, c * 128:(c + 1) * 128], "(fc p) n -> p fc n", p=128)

        def cast_cols(u, li, c0, n):
            cast(u, li * 1024, n, w_in[:, c0:c0 + n], "(kc p) n -> p kc n", p=128)

        def cast_cols_part(u, li, noff, c0, n):
            srcv = w_in[:, c0:c0 + n].rearrange("(kc p) n -> p kc n", p=128)
            dst = WS[u][:, li * 1024:(li + 1) * 1024].rearrange("p (a b) -> p a b", b=128)[:, :, noff:noff + n]
            DMA("pool", "cast", dst, srcv, r=[], w=[wspart(u)])

        MEMSET("pool", VA[:, :, :, 64:65], 1.0, r=[], w=[("VA",)])
        MEMSET("pool", VA[:, 0:1, :, 0:64], 0.0, r=[], w=[("VA",)])
        MEMSET("pool", KA[:, 0:128], 0.0, r=[], w=[("KA",)])
        MEMSET("pool", VBc[:, :, :, 64:65], 1.0, r=[], w=[("VBc",)])
        MEMSET("pool", VM[:, :, :, 64:65], 1.0, r=[], w=[("VM",)])
        MEMSET("pool", KmT[:, :, :], 0.0, r=[], w=[("KmT",)])
        def early_casts():
            cast_ffn(1)
            for li in range(3):
                cast_cols(U_WB, li, 640 + li * 128, 128)
            cast_cols(U_WB, 3, 1024, 128)
            cast_cols(U_WC, 0, 1152, 128)
            cast_cols(U_WC, 1, 1280, 128)
            cast_cols(U_WC, 2, 1792, 128)
            cast_cols(U_WC, 3, 1920, 128)
            for (noff, c0, n) in ((0, 512, 128), (128, 1408, 384)):
                srcv = w_in[:, c0:c0 + n].rearrange("(kc p) n -> p kc n", p=128)
                dst = WS[U_WV][:, 0:4096].rearrange("p (a b) -> p a b", b=512)[:, :, noff:noff + n]
                DMA("pool", "cast", dst, srcv, r=[], w=[wspart(U_WV)])
            for j in range(3):
                cast_cols_part(U_WA, j, 0, 64 * j, 64)
                cast_cols_part(U_WA, j, 64, 64 * (3 + j), 64)
            cast_cols(U_WA, 3, 384, 128)

        def late_casts_all():
            cast(U_MKV, 0, 512, w_mkv, "(kc p) n -> p kc n", p=128)
            for c in range(8):
                for rr in range(3):
                    cast(U_MIX + c, rr * 1024, 128, w_in[:, 2048 + rr * 1024 + c * 128: 2048 + rr * 1024 + (c + 1) * 128],
                         "(kc p) n -> p kc n", p=128)
                cast(U_MIX + c, 3072, 128, w_pswa[:, c * 128:(c + 1) * 128], "(fc p) n -> p fc n", p=128)
                cast(U_MIX + c, 3072 + 384, 128, w_pmoba[:, c * 128:(c + 1) * 128], "(fc p) n -> p fc n", p=128)
                cast(U_MIX + c, 3072 + 768, 128, w_pmem[:, c * 128:(c + 1) * 128], "(fc p) n -> p fc n", p=128)
            for hh in range(2):
                cast(U_WOUT + hh, 0, 512, w_out[:, hh * 512:(hh + 1) * 512], "(kc p) n -> p kc n", p=128)
            cast_ffn(2)

        UNIT_NEL = {}
        for k in (1, 2):
            for j in range(11):
                UNIT_NEL[U_FFN[k] + j] = 4096
            for c in range(8):
                UNIT_NEL[U_FFN[k] + 11 + c] = 2816
        for u in (U_WA, U_WB, U_WC, U_WV, U_MKV, U_WOUT, U_WOUT + 1):
            UNIT_NEL[u] = 4096
        for c in range(8):
            UNIT_NEL[U_MIX + c] = 4096

        wcnt = [0]

        def load_unit(u):
            slot = wcnt[0] % NSLOT
            wcnt[0] += 1
            n = UNIT_NEL[u]
            DMA("sp", "w", wsl[slot][:, 0:n], WS[u][:, 0:n], r=list(ws_parts[u]), w=[("w", slot)])
            return slot

        DMA("pool", "init", cf[:, :], cf_d[:, :], r=[], w=[("cf",)])
        DMA("pool", "init", cb[:, :], cb_d[:, :], r=[], w=[("cb",)])
        RC = [("cf",), ("cb",)]
        for _e in ("pe", "act", "dve", "pool", "sp"):
            S.add(_e, None, r=RC, w=[])
        for gi, (gn, bn) in enumerate((("g1", "b1"), ("g2", "b2"))):
            TS("dve", prm[:, gi * 16:gi * 16 + 8], cfc(gn, 0, 8), -1.0, None, ALU.mult, None, r=RC, w=[("prm",)])
            TS("dve", prm[:, gi * 16 + 8:gi * 16 + 16], cfc(bn, 0, 8), ALPHA, None, ALU.mult, None, r=RC, w=[("prm",)])
        TT("dve", prm[:, 32:38], cfc("sinks", 0, 6), cfc("sinkc", 0, 6), ALU.add, r=RC, w=[("prm",)])
        ACTF(prm[:, 32:38], prm[:, 32:38], AF.Exp, r=[("prm",)], w=[("prm",)])
        TS("dve", prm[:, 40:46], cfc("swaprev", 0, 6), cfc("cmask", 0, 1), None, ALU.add, None, r=RC, w=[("prm",)])

        def ngc(gi, c):
            return prm[:, gi * 16 + c:gi * 16 + c + 1]

        def abc(gi, c):
            return prm[:, gi * 16 + 8 + c:gi * 16 + 8 + c + 1]

        sinkq = prm[:, 32:38]
        swaprev0 = prm[:, 40:46]

        def init_mem():
            for s2 in range(2):
                DMA("pool", "xl", xs[s2][:, :], mem[s2 * 128:(s2 + 1) * 128, :], r=[], w=[("Otok", 2 + s2)])
                for half in range(2):
                    b = newps()
                    for c4 in range(4):
                        c = half * 4 + c4
                        TR(PS[b][:, c4 * 128:(c4 + 1) * 128], xs[s2][:, c * 128:(c + 1) * 128], r=[("Otok", 2 + s2)] + RC, w=[("ps", b)])
                    CP("dve", memT[:, half * 4:half * 4 + 4, s2 * 128:(s2 + 1) * 128],
                       PS[b][:, :].rearrange("p (c n) -> p c n", n=128), r=[("ps", b)], w=[("memT",)])
            slot = load_unit(U_MKV)
            wv = wsl[slot][:, 0:4096].rearrange("p (kc n) -> p kc n", n=512)
            for j in range(2):
                b = newps()
                for kc in range(8):
                    MM(PS[b][:, 0:256], wv[:, kc, j * 128:(j + 1) * 128], memT[:, kc, :], kc == 0, kc == 7,
                       r=[("w", slot), ("memT",)], w=[("ps", b)])
                ACTF(KM[:, j, :], PS[b][:, 0:256], AF.Identity, r=[("ps", b)] + RC, w=[("KM",)], bias=cfc("bmkfm", j, 1))
            for s2 in range(2):
                b = newps()
                for kc in range(8):
                    MM(PS[b][:, 0:256], memT[:, kc, s2 * 128:(s2 + 1) * 128], wv[:, kc, 256:512], kc == 0, kc == 7,
                       r=[("w", slot), ("memT",)], w=[("ps", b)])
                TT("dve", VM[:, s2, :, 0:64], PS[b][:, 0:256].rearrange("p (h d) -> p h d", d=64),
                   cf[:, C["bmkv"]:C["bmkv"] + 256].rearrange("p (h d) -> p h d", d=64), ALU.add,
                   r=[("ps", b)] + RC, w=[("VM",)])

        sgc = [0]
        sqc = [0]
        xsc = [0]
        ptc = [0]

        def load_x(src, t):
            for s4 in range(4):
                i = xsc[0] % 2
                xsc[0] += 1
                DMA("pool", "xl", xs[i][:, :], src[t * T + s4 * 128: t * T + (s4 + 1) * 128, :], r=[], w=[("Otok", 2 + i)])
                for half in range(2):
                    b = newps()
                    for c4 in range(4):
                        c = half * 4 + c4
                        TR(PS[b][:, c4 * 128:(c4 + 1) * 128], xs[i][:, c * 128:(c + 1) * 128], r=[("Otok", 2 + i)], w=[("ps", b)])
                    pv = PS[b][:, :].rearrange("p (c n) -> p c n", n=128)
                    ACTF(xa[:, half * 4:half * 4 + 4, s4 * 128:(s4 + 1) * 128], pv, AF.Copy,
                         r=[("ps", b)], w=[("xa", c) for c in range(half * 4, half * 4 + 4)], scale=ALPHA)
                    CP("dve", xb[:, half * 4:half * 4 + 4, s4 * 128:(s4 + 1) * 128], pv,
                       r=[("ps", b)], w=[("xb",)])

        def ffn(k):
            base = U_FFN[k]
            for j in range(11):
                slot = load_unit(base + j)
                for fcl in range(2):
                    fc = 2 * j + fcl
                    bg, bu = newps(), newps()
                    for gu, b in ((0, bg), (1, bu)):
                        for kc in range(8):
                            off = ((fcl * 2 + gu) * 8 + kc) * 128
                            MM(PS[b][:, :], wsl[slot][:, off:off + 128], xb[:, kc, :], kc == 0, kc == 7,
                               r=[("w", slot), ("xb",)], w=[("ps", b)])
                    i = sgc[0] % 2
                    sgc[0] += 1
                    ACTF(sg[i][:, :], PS[bg][:, :], AF.Silu, r=[("ps", bg)], w=[("sg", i)])
                    TT("dve", hT[:, fc, :], sg[i][:, :], PS[bu][:, :], ALU.mult, r=[("sg", i), ("ps", bu)], w=[("hT", fc)])
            for c in range(8):
                slot = load_unit(base + 11 + c)
                b = newps()
                for fc in range(22):
                    MM(PS[b][:, :], wsl[slot][:, fc * 128:(fc + 1) * 128], hT[:, fc, :], fc == 0, fc == 21,
                       r=[("w", slot), ("hT", fc)], w=[("ps", b)])
                STT(xa[:, c, :], PS[b][:, :], 0.5, xa[:, c, :], ALU.mult, ALU.add, r=[("ps", b), ("xa", c)], w=[("xa", c)])

        def ln_fm(gi):
            gname = "g1" if gi == 0 else "g2"
            bname = "b1" if gi == 0 else "b2"
            bs, bq = newps(), newps()
            for c in range(8):
                i = sqc[0] % 2
                sqc[0] += 1
                ACTF(sq[i][:, :], xa[:, c, :], AF.Square, r=[("xa", c)], w=[("sg", i)])
                MM(PS[bs][0:1, :], ones_col, xa[:, c, :], c == 0, c == 7, r=[("xa", c)], w=[("ps", bs)])
                MM(PS[bq][0:1, :], ones_col, sq[i][:, :], c == 0, c == 7, r=[("sg", i)], w=[("ps", bq)])
            RS = [("st",)]
            TS("dve", st[0:1, 0, :], PS[bs][0:1, :], 1.0 / 1024, None, ALU.mult, None, r=[("ps", bs)], w=RS)
            TT("dve", st[0:1, 1, :], st[0:1, 0, :], st[0:1, 0, :], ALU.mult, r=RS, w=RS)
            STT(st[0:1, 1, :], PS[bq][0:1, :], 1.0 / 1024, st[0:1, 1, :], ALU.mult, ALU.subtract, r=[("ps", bq)] + RS, w=RS)
            ACTF(st[0:1, 2, :], st[0:1, 1, :], AF.Sqrt, r=RS, w=RS, bias=eps_row)
            S.add("dve", lambda e: e.reciprocal(out=st[0:1, 2, :], in_=st[0:1, 2, :]), r=RS, w=RS)
            TT("dve", st[0:1, 3, :], st[0:1, 0, :], st[0:1, 2, :], ALU.mult, r=RS, w=RS)
            b1, b2 = newps(), newps()
            MM(PS[b1][:, :], ones_row, st[0:1, 2, :], True, True, r=RS, w=[("ps", b1)])
            MM(PS[b2][:, :], ones_row, st[0:1, 3, :], True, True, r=RS, w=[("ps", b2)])
            for c in range(8):
                STT(xa[:, c, :], xa[:, c, :], cfc(gname, c, 1), PS[b1][:, :], ALU.mult, ALU.mult,
                    r=[("xa", c), ("ps", b1)], w=[("xa", c)])
                STT(xa[:, c, :], PS[b2][:, :], ngc(gi, c), xa[:, c, :], ALU.mult, ALU.add,
                    r=[("xa", c), ("ps", b2)], w=[("xa", c)])
                TS("dve", xb[:, c, :], xa[:, c, :], cfc(bname, c, 1), None, ALU.add, None, r=[("xa", c)], w=[("xb",)])
                ACTF(xa[:, c, :], xa[:, c, :], AF.Identity, r=[("xa", c), ("xb",)], w=[("xa", c)], bias=abc(gi, c), scale=ALPHA)

        def proj_fm(slot, li, dst, bias_idx, r_extra=(), w=()):
            b = newps()
            wv = wsl[slot][:, li * 1024:(li + 1) * 1024].rearrange("p (kc n) -> p kc n", n=128)
            for kc in range(8):
                MM(PS[b][:, :], wv[:, kc, :], xb[:, kc, :], kc == 0, kc == 7, r=[("w", slot), ("xb",)], w=[("ps", b)])
            ACTF(dst, PS[b][:, :], AF.Identity, r=[("ps", b)], w=list(w), bias=cfc("binfm", bias_idx, 1))
            return b

        def kb_chunk(slot, li, j, bias_idx, nblk0):
            b = proj_fm(slot, li, KBc[:, j, :], bias_idx, w=[("KBc",)])
            S.add("dve", lambda e: e.tensor_reduce(out=lmv[:, 0:2], in_=PS[b][:, :].rearrange("p (a k) -> p a k", k=256),
                                                   axis=AX.X, op=ALU.add), r=[("ps", b)], w=[("lmv",)])
            TS("dve", KmT[:, j, nblk0:nblk0 + 2], lmv[:, 0:2], 1.0 / 256, cfc("binfm", bias_idx, 1), ALU.mult, ALU.add,
               r=[("lmv",)], w=[("KmT",)])

        def v_proj(t, main, slot):
            wv = wsl[slot][:, 0:4096].rearrange("p (kc n) -> p kc n", n=512)
            for s4 in range(4):
                b = newps()
                for kc in range(8):
                    MM(PS[b][:, :], xb[:, kc, s4 * 128:(s4 + 1) * 128], wv[:, kc, :], kc == 0, kc == 7,
                       r=[("w", slot), ("xb",)], w=[("ps", b)])
                va_idx = None
                if main:
                    va_idx = 4 * t + s4 + 1
                elif t == NT - 1 and s4 == 3:
                    va_idx = 0
                if va_idx is not None:
                    TT("dve", VA[:, va_idx, :, 0:64], PS[b][:, 0:128].rearrange("p (g d) -> p g d", d=64),
                       cf[:, C["binv"]:C["binv"] + 128].rearrange("p (g d) -> p g d", d=64), ALU.add,
                       r=[("ps", b)], w=[("VA",)])
                TT("dve", VBc[:, s4, :, 0:64], PS[b][:, 128:512].rearrange("p (h d) -> p h d", d=64),
                   cf[:, C["binv"] + 128:C["binv"] + 512].rearrange("p (h d) -> p h d", d=64), ALU.add,
                   r=[("ps", b)], w=[("VBc",)])

        def store_kv(koff):
            n0 = koff // 256
            DMA("pool", "kvs", KTs[:, :, koff:koff + T].rearrange("j p k -> p j k"), KBc[:, :, :],
                r=[("KBc",)], w=[("kvd", n0), ("kvd", n0 + 1)])
            DMA("pool", "kvs", Vs[koff:koff + T, :].rearrange("(s p) f -> p s f", p=128),
                VBc[:, :, :, :].rearrange("p s h d -> p s (h d)"),
                r=[("VBc",)], w=[("kvd", n0), ("kvd", n0 + 1)])

        def other_tile(t, skip_load=False):
            if not skip_load:
                load_x(x_other, t)
            if upto < 2:
                return
            ffn(1)
            if upto < 3:
                return
            ln_fm(0)
            if upto < 4:
                return
            slot = load_unit(U_WB)
            kb_chunk(slot, 3, 0, 7, 2 * t)
            slot = load_unit(U_WC)
            kb_chunk(slot, 0, 1, 8, 2 * t)
            kb_chunk(slot, 1, 2, 9, 2 * t)
            if t == NT - 1:
                slot = load_unit(U_WA)
                b = newps()
                wv = wsl[slot][:, 3 * 1024:4 * 1024].rearrange("p (kc n) -> p kc n", n=128)
                for kc in range(8):
                    MM(PS[b][:, 0:128], wv[:, kc, :], xb[:, kc, 384:512], kc == 0, kc == 7, r=[("w", slot), ("xb",)], w=[("ps", b)])
                ACTF(KA[:, 0:128], PS[b][:, 0:128], AF.Identity, r=[("ps", b)], w=[("KA",)], bias=cfc("binfm", 3, 1))
            if upto < 5:
                return
            slot = load_unit(U_WV)
            v_proj(t, False, slot)
            if upto < 6:
                return
            store_kv(t * T)

        def nextpt():
            i = ptc[0] % NPT
            ptc[0] += 1
            return i

        def swa(t):
            for s4 in range(4):
                J = 4 * t + s4
                bo = newps()
                for h in range(6):
                    g, j = h // 3, h % 3
                    rows = slice(g * 64, g * 64 + 64)
                    q = QA[rows, j, s4 * 128:(s4 + 1) * 128]
                    b = newps()
                    MM(PS[b][:, 0:128], KA[rows, 128 * J:128 * J + 128], q, True, False, r=[("KA",), ("QA",)], w=[("ps", b)])
                    MM(PS[b][:, 0:128], identb, anti, False, True, r=[], w=[("ps", b)])
                    MM(PS[b][:, 128:256], KA[rows, 128 * (J + 1):128 * (J + 1) + 128], q, True, False, r=[("KA",), ("QA",)], w=[("ps", b)])
                    MM(PS[b][:, 128:256], identb, causal, False, True, r=[], w=[("ps", b)])
                    pi = nextpt()
                    pb = swaprev0[:, h:h + 1] if (t == 0 and s4 == 0) else cfc("swaprev", h, 1)
                    ACTF(PT[pi][:, 0:128], PS[b][:, 0:128], AF.Exp, r=[("ps", b)], w=[("PT", pi)], bias=pb, scale=0.125)
                    ACTF(PT[pi][:, 128:256], PS[b][:, 128:256], AF.Exp, r=[("ps", b)], w=[("PT", pi)], bias=cfc("swacur", h, 1), scale=0.125)
                    MM(PS[bo][:, h * 65:(h + 1) * 65], PT[pi][:, 0:128], VA[:, J, g, :], True, False, r=[("PT", pi), ("VA",)], w=[("ps", bo)])
                    MM(PS[bo][:, h * 65:(h + 1) * 65], PT[pi][:, 128:256], VA[:, J + 1, g, :], False, True, r=[("PT", pi), ("VA",)], w=[("ps", bo)])
                pv = PS[bo][:, 0:390].rearrange("p (h d) -> p h d", d=65)
                TT("dve", den[:, 0:6], pv[:, :, 64], sinkq, ALU.add, r=[("ps", bo), ("prm",)], w=[("den",)])
                S.add("dve", lambda e: e.reciprocal(out=den[:, 0:6], in_=den[:, 0:6]), r=[("den",)], w=[("den",)])
                TT("dve", Otok[:, s4, 0:384].rearrange("p (h d) -> p h d", d=64), pv[:, :, 0:64],
                   den[:, 0:6].unsqueeze(2).to_broadcast([128, 6, 64]), ALU.mult, r=[("ps", bo), ("den",)], w=[("Otok", s4)])

        def memattn(t):
            for hm in range(4):
                j, rows = hm // 2, slice((hm % 2) * 64, (hm % 2) * 64 + 64)
                pis = []
                for sub in range(2):
                    b = newps()
                    MM(PS[b][:, :], KM[rows, j, sub * 128:(sub + 1) * 128], QM[rows, j, :], True, True, r=[("KM",), ("QM",)], w=[("ps", b)])
                    pi = nextpt()
                    ACTF(PT[pi][:, :], PS[b][:, :], AF.Exp, r=[("ps", b)], w=[("PT", pi)], scale=0.125)
                    pis.append(pi)
                bo = newps()
                for s4 in range(4):
                    for sub in range(2):
                        MM(PS[bo][:, s4 * 65:(s4 + 1) * 65], PT[pis[sub]][:, s4 * 128:(s4 + 1) * 128], VM[:, sub, hm, :],
                           sub == 0, sub == 1, r=[("PT", pis[sub]), ("VM",)], w=[("ps", bo)])
                pv = PS[bo][:, 0:260].rearrange("p (s d) -> p s d", d=65)
                S.add("dve", lambda e, pv=pv: e.reciprocal(out=den[:, 8:12], in_=pv[:, :, 64]), r=[("ps", bo)], w=[("den",)])
                TT("dve", Otok[:, :, 768 + hm * 64:768 + (hm + 1) * 64], pv[:, :, 0:64],
                   den[:, 8:12].unsqueeze(2).to_broadcast([128, 4, 64]), ALU.mult, r=[("ps", bo), ("den",)],
                   w=[("Otok", s4) for s4 in range(4)])

        kvlc = [0]

        def moba(t):
            n1 = 16 + 2 * t
            n2 = n1 + 1
            for s4 in range(4):
                i_loc = 2 * t + s4 // 2
                vb = cf[:, C["vbias"] + i_loc * 32:C["vbias"] + (i_loc + 1) * 32]
                for par in range(2):
                    bgp = newps()
                    rows = slice(par * 64, par * 64 + 64)
                    for j in range(3):
                        MM(PS[bgp][:, j * 32:(j + 1) * 32], QB[rows, j, s4 * 128:(s4 + 1) * 128], KmT[rows, j, :], True, True,
                           r=[("QB",), ("KmT",)], w=[("ps", bgp)])
                    gbv = gb[:, s4, :, :].rearrange("p (j two) n -> p j two n", two=2)[:, :, par, :]
                    TT("dve", gbv, PS[bgp][:, 0:96].rearrange("p (j n) -> p j n", n=32),
                       vb.unsqueeze(1).to_broadcast([128, 3, 32]), ALU.add, r=[("ps", bgp)], w=[("gb",)])
                for h in range(6):
                    S.add("dve", lambda e, s4=s4, h=h: e.max(out=t8[:, s4 * 6 + h, :], in_=gb[:, s4, h, :]), r=[("gb",)], w=[("t8",)])
            TS("dve", thr[:, :], t8[:, :, 2], -1e29, None, ALU.max, None, r=[("t8",)], w=[("thr",)])
            TT("dve", fT[:, :, :, :].rearrange("p s h n -> p (s h) n"), gb[:, :, :, :].rearrange("p s h n -> p (s h) n"),
               thr[:, :].unsqueeze(2).to_broadcast([128, 24, 32]), ALU.is_ge, r=[("gb",), ("thr",)], w=[("fT",)])
            for s4 in range(4):
                bq, par = s4 // 2, s4 % 2
                o = C["tg"] + (bq * 2 + par) * 6 * 46
                tgv = cf[:, o:o + 6 * 46].rearrange("p (h u) -> p h u", u=46)[:, :, 14 - 2 * t:14 - 2 * t + 32]
                TT("dve", fT[:, s4, :, :], fT[:, s4, :, :], tgv, ALU.mult, r=[("fT",)], w=[("fT",)])
            MEMSET("dve", fT[:, 0:2, :, n1:n1 + 1], 1.0, r=[("fT",)], w=[("fT",)])
            MEMSET("dve", fT[:, 2:4, :, n2:n2 + 1], 1.0, r=[("fT",)], w=[("fT",)])
            MEMSET("pool", acc[:, :, :, :], 0.0, r=[], w=[("acc", a_, b_) for a_ in range(4) for b_ in range(6)])
            if upto < 12.5:
                return

            def pv_acc(h, n, pia, pib, va, vb_, s_list, b_from, vres):
                bo = newps()
                for s4 in s_list:
                    o = PS[bo][:, s4 * 65:(s4 + 1) * 65]
                    useb = b_from[s4]
                    MM(o, PT[pia][:, s4 * 128:(s4 + 1) * 128], va, True, not useb, r=[("PT", pia)] + vres, w=[("ps", bo)])
                    if useb:
                        MM(o, PT[pib][:, s4 * 128:(s4 + 1) * 128], vb_, False, True, r=[("PT", pib)] + vres, w=[("ps", bo)])
                for s4 in s_list:
                    STT(acc[:, s4, h, :], PS[bo][:, s4 * 65:(s4 + 1) * 65], fT[:, s4, h, n:n + 1], acc[:, s4, h, :],
                        ALU.mult, ALU.add, r=[("ps", bo), ("fT",), ("acc", s4, h)], w=[("acc", s4, h)])

            for n in range(16 if skip_first else 0, n1):
                i = kvlc[0] % 3
                kvlc[0] += 1
                DMA("pool", "kvl", KS[i][:, :, :], KTs[:, :, n * 256:(n + 1) * 256].rearrange("j p k -> p j k"),
                    r=[("kvd", n)], w=[("KS", i)])
                DMA("pool", "kvl", VS[i][:, :, :, :].rearrange("p s h d -> p s (h d)"),
                    Vs[n * 256:(n + 1) * 256, :].rearrange("(s p) f -> p s f", p=128), r=[("kvd", n)], w=[("VS", i)])
                for h in range(6):
                    j, rows = h // 2, slice((h % 2) * 64, (h % 2) * 64 + 64)
                    ba, bb = newps(), newps()
                    MM(PS[ba][:, :], KS[i][rows, j, 0:128], QB[rows, j, :], True, True, r=[("KS", i), ("QB",)], w=[("ps", ba)])
                    MM(PS[bb][:, :], KS[i][rows, j, 128:256], QB[rows, j, :], True, True, r=[("KS", i), ("QB",)], w=[("ps", bb)])
                    pia, pib = nextpt(), nextpt()
                    ACTF(PT[pia][:, :], PS[ba][:, :], AF.Exp, r=[("ps", ba)], w=[("PT", pia)], bias=cfc("mb0", h, 1), scale=0.125)
                    ACTF(PT[pib][:, :], PS[bb][:, :], AF.Exp, r=[("ps", bb)], w=[("PT", pib)], bias=cfc("mb1", h, 1), scale=0.125)
                    pv_acc(h, n, pia, pib, VS[i][:, 0, h, :], VS[i][:, 1, h, :], [0, 1, 2, 3], [True] * 4, [("VS", i)])
            for h in range(6):
                j, rows = h // 2, slice((h % 2) * 64, (h % 2) * 64 + 64)
                ba, bb = newps(), newps()
                MM(PS[ba][:, 0:128], KBc[rows, j, 0:128], QB[rows, j, 0:128], True, False, r=[("KBc",), ("QB",)], w=[("ps", ba)])
                MM(PS[ba][:, 0:128], identb, causal, False, True, r=[], w=[("ps", ba)])
                MM(PS[ba][:, 128:512], KBc[rows, j, 0:128], QB[rows, j, 128:512], True, True, r=[("KBc",), ("QB",)], w=[("ps", ba)])
                MM(PS[bb][:, 128:256], KBc[rows, j, 128:256], QB[rows, j, 128:256], True, False, r=[("KBc",), ("QB",)], w=[("ps", bb)])
                MM(PS[bb][:, 128:256], identb, causal, False, True, r=[], w=[("ps", bb)])
                MM(PS[bb][:, 256:512], KBc[rows, j, 128:256], QB[rows, j, 256:512], True, True, r=[("KBc",), ("QB",)], w=[("ps", bb)])
                pia, pib = nextpt(), nextpt()
                ACTF(PT[pia][:, 0:128], PS[ba][:, 0:128], AF.Exp, r=[("ps", ba)], w=[("PT", pia)], bias=cfc("mb1", h, 1), scale=0.125)
                ACTF(PT[pia][:, 128:512], PS[ba][:, 128:512], AF.Exp, r=[("ps", ba)], w=[("PT", pia)], bias=cfc("mb0", h, 1), scale=0.125)
                ACTF(PT[pib][:, 128:512], PS[bb][:, 128:512], AF.Exp, r=[("ps", bb)], w=[("PT", pib)], bias=cfc("mb1", h, 1), scale=0.125)
                pv_acc(h, n1, pia, pib, VBc[:, 0, h, :], VBc[:, 1, h, :], [0, 1, 2, 3], [False, True, True, True], [("VBc",)])
                ba, bb = newps(), newps()
                MM(PS[ba][:, 256:384], KBc[rows, j, 256:384], QB[rows, j, 256:384], True, False, r=[("KBc",), ("QB",)], w=[("ps", ba)])
                MM(PS[ba][:, 256:384], identb, causal, False, True, r=[], w=[("ps", ba)])
                MM(PS[ba][:, 384:512], KBc[rows, j, 256:384], QB[rows, j, 384:512], True, True, r=[("KBc",), ("QB",)], w=[("ps", ba)])
                MM(PS[bb][:, 384:512], KBc[rows, j, 384:512], QB[rows, j, 384:512], True, False, r=[("KBc",), ("QB",)], w=[("ps", bb)])
                MM(PS[bb][:, 384:512], identb, causal, False, True, r=[], w=[("ps", bb)])
                pia, pib = nextpt(), nextpt()
                ACTF(PT[pia][:, 256:384], PS[ba][:, 256:384], AF.Exp, r=[("ps", ba)], w=[("PT", pia)], bias=cfc("mb1", h, 1), scale=0.125)
                ACTF(PT[pia][:, 384:512], PS[ba][:, 384:512], AF.Exp, r=[("ps", ba)], w=[("PT", pia)], bias=cfc("mb0", h, 1), scale=0.125)
                ACTF(PT[pib][:, 384:512], PS[bb][:, 384:512], AF.Exp, r=[("ps", bb)], w=[("PT", pib)], bias=cfc("mb1", h, 1), scale=0.125)
                pv_acc(h, n2, pia, pib, VBc[:, 2, h, :], VBc[:, 3, h, :], [2, 3], [False, False, False, True], [("VBc",)])
            if upto < 12.8:
                return
            for s4 in range(4):
                S.add("dve", lambda e, s4=s4: e.reciprocal(out=den[:, 16:22], in_=acc[:, s4, :, 64]), r=[("acc", s4, b_) for b_ in range(6)], w=[("den",)])
                TT("dve", Otok[:, s4, 384:768].rearrange("p (h d) -> p h d", d=64), acc[:, s4, :, 0:64],
                   den[:, 16:22].unsqueeze(2).to_broadcast([128, 6, 64]), ALU.mult, r=[("acc", s4, b_) for b_ in range(6)] + [("den",)], w=[("Otok", s4)])

        def o_transpose():
            for c8 in range(8):
                b = newps()
                for s4 in range(4):
                    TR(PS[b][:, s4 * 128:(s4 + 1) * 128], Otok[:, s4, c8 * 128:(c8 + 1) * 128], r=[("Otok", s4)], w=[("ps", b)])
                CP("dve", OT[:, c8, :], PS[b][:, :], r=[("ps", b)], w=R_OT)

        def mixing():
            for c in range(8):
                slot = load_unit(U_MIX + c)
                gv = wsl[slot][:, 0:3072].rearrange("p (r kc n) -> p r kc n", kc=8, n=128)
                pw = wsl[slot][:, 3072:4096].rearrange("p (fc n) -> p fc n", n=128)
                fcs = [(0, 3), (3, 6), (6, 8)]
                for rr in range(3):
                    bgp, by = newps(), newps()
                    for kc in range(8):
                        MM(PS[bgp][:, :], gv[:, rr, kc, :], xb[:, kc, :], kc == 0, kc == 7, r=[("w", slot), ("xb",)], w=[("ps", bgp)])
                    f0, f1 = fcs[rr]
                    for fc in range(f0, f1):
                        MM(PS[by][:, :], pw[:, fc, :], OT[:, fc, :], fc == f0, fc == f1 - 1, r=[("w", slot)] + R_OT, w=[("ps", by)])
                    i = sgc[0] % 2
                    sgc[0] += 1
                    ACTF(sg[i][:, :], PS[bgp][:, :], AF.Sigmoid, r=[("ps", bgp)], w=[("sg", i)], bias=cfc("binfm", 12 + rr * 8 + c, 1))
                    TT("dve", mt[rr][:, :], sg[i][:, :], PS[by][:, :], ALU.mult, r=[("sg", i), ("ps", by)], w=R_MT[rr])
                TT("pool", mt[0][:, :], mt[0][:, :], mt[1][:, :], ALU.add, r=R_MT[0] + R_MT[1], w=R_MT[0])
                TT("pool", mixed[:, c, :], mt[0][:, :], mt[2][:, :], ALU.add, r=R_MT[0] + R_MT[2], w=R_MIXED)
            for hh in range(2):
                slot = load_unit(U_WOUT + hh)
                wv = wsl[slot][:, 0:4096].rearrange("p (kc n) -> p kc n", n=512)
                for c4 in range(4):
                    c = hh * 4 + c4
                    b = newps()
                    for kc in range(8):
                        MM(PS[b][:, :], wv[:, kc, c4 * 128:(c4 + 1) * 128], mixed[:, kc, :], kc == 0, kc == 7,
                           r=[("w", slot)] + R_MIXED, w=[("ps", b)])
                    TT("dve", xa[:, c, :], PS[b][:, :], xa[:, c, :], ALU.add, r=[("ps", b), ("xa", c)], w=[("xa", c)])

        oc = [0]

        def ln3_out(t):
            g3 = cf[:, C["g3"]:C["g3"] + 1024]
            b3 = cf[:, C["b3"]:C["b3"] + 1024]
            for s4 in range(4):
                i = oc[0] % 2
                oc[0] += 1
                RO, RT = [("Otok", i)], [("Otok", 2 + i)]
                tmp = Otok[:, 2 + i, :]
                for half in range(2):
                    b = newps()
                    for c4 in range(4):
                        c = half * 4 + c4
                        TR(PS[b][:, c4 * 128:(c4 + 1) * 128], xa[:, c, s4 * 128:(s4 + 1) * 128], r=[("xa", c)], w=[("ps", b)])
                    ACTF(ostg[i][:, half * 512:(half + 1) * 512], PS[b][:, :], AF.Copy, r=[("ps", b)], w=RO)
                S.add("dve", lambda e, i=i: e.tensor_reduce(out=lmv[:, 2:3], in_=ostg[i][:, :], axis=AX.X, op=ALU.add), r=RO, w=[("lmv",)])
                TS("dve", lmv[:, 2:3], lmv[:, 2:3], 1.0 / 1024, None, ALU.mult, None, r=[("lmv",)], w=[("lmv",)])
                TS("dve", ostg[i][:, :], ostg[i][:, :], lmv[:, 2:3], None, ALU.subtract, None, r=RO + [("lmv",)], w=RO)
                ACTF(tmp, ostg[i][:, :], AF.Square, r=RO, w=RT)
                S.add("dve", lambda e, tmp=tmp: e.tensor_reduce(out=lmv[:, 3:4], in_=tmp, axis=AX.X, op=ALU.add), r=RT, w=[("lmv",)])
                ACTF(lmv[:, 3:4], lmv[:, 3:4], AF.Sqrt, r=[("lmv",)], w=[("lmv",)], bias=eps_col, scale=1.0 / 1024)
                S.add("dve", lambda e: e.reciprocal(out=lmv[:, 3:4], in_=lmv[:, 3:4]), r=[("lmv",)], w=[("lmv",)])
                STT(ostg[i][:, :], ostg[i][:, :], lmv[:, 3:4], g3, ALU.mult, ALU.mult, r=RO + [("lmv",)], w=RO)
                TT("pool", ostg[i][:, :], ostg[i][:, :], b3, ALU.add, r=RO, w=RO)
                DMA("pool", "out", out[t * T + s4 * 128:t * T + (s4 + 1) * 128, :], ostg[i][:, :], r=RO, w=[("outd",)])

        def main_tile(t):
            load_x(x_main, t)
            ffn(1)
            ln_fm(0)
            slot = load_unit(U_WA)
            for j in range(3):
                proj_fm(slot, j, QA[:, j, :], j, w=[("QA",)])
            proj_fm(slot, 3, KA[:, 128 + t * T:128 + (t + 1) * T], 3, w=[("KA",)])
            slot = load_unit(U_WB)
            for j in range(3):
                proj_fm(slot, j, QB[:, j, :], 4 + j, w=[("QB",)])
            kb_chunk(slot, 3, 0, 7, 16 + 2 * t)
            slot = load_unit(U_WC)
            kb_chunk(slot, 0, 1, 8, 16 + 2 * t)
            kb_chunk(slot, 1, 2, 9, 16 + 2 * t)
            proj_fm(slot, 2, QM[:, 0, :], 10, w=[("QM",)])
            proj_fm(slot, 3, QM[:, 1, :], 11, w=[("QM",)])
            slot = load_unit(U_WV)
            v_proj(t, True, slot)
            if t < NT - 1:
                store_kv(4096 + t * T)
            if upto < 11:
                return
            swa(t)
            if upto < 12:
                return
            memattn(t)
            if upto < 12.2:
                return
            moba(t)
            if upto < 14:
                return
            o_transpose()
            if upto < 15:
                return
            mixing()
            if upto < 16:
                return
            ln_fm(1)
            if upto < 17:
                return
            ffn(2)
            if upto < 18:
                return
            ln3_out(t)

        if n_other > 0:
            load_x(x_other, 0)
        early_casts()
        for t in range(n_other):
            other_tile(t, skip_load=(t == 0))
            if t == 0:
                late_casts_all()
        if n_other == 0:
            late_casts_all()
        init_mem()
        for t in range(n_main):
            main_tile(t)
        if dbg_d is not None:
            bufs = {"xa": (xa[:, :, :].rearrange("p a b -> p (a b)"), [("xa", c) for c in range(8)]),
                    "xb": (xb[:, :, :].rearrange("p a b -> p (a b)"), [("xb",)]),
                    "Otok": (Otok[:, :, :].rearrange("p a b -> p (a b)"), [("Otok", i) for i in range(4)]),
                    "KBc": (KBc[:, :, :].rearrange("p a b -> p (a b)"), [("KBc",)]),
                    "VBc": (VBc[:, :, :, :].rearrange("p a b c -> p (a b c)"), [("VBc",)]),
                    "QB": (QB[:, :, :].rearrange("p a b -> p (a b)"), [("QB",)]),
                    "fT": (fT[:, :, :, :].rearrange("p a b c -> p (a b c)"), [("fT",)]),
                    "gb": (gb[:, :, :, :].rearrange("p a b c -> p (a b c)"), [("gb",)]),
                    "acc": (acc[:, :, :, :].rearrange("p a b c -> p (a b c)"), [("acc", a_, b_) for a_ in range(4) for b_ in range(6)]),
                    "KM": (KM[:, :, :].rearrange("p a b -> p (a b)"), [("KM",)]),
                    "VM": (VM[:, :, :, :].rearrange("p a b c -> p (a b c)"), [("VM",)]),
                    "hT": (hT[:, :, :].rearrange("p a b -> p (a b)"), [("hT", i) for i in range(22)]),
                    "KmT": (KmT[:, :, :].rearrange("p a b -> p (a b)"), [("KmT",)]),
                    "prm": (prm[:, :], [("prm",)]),
                    }
            ap2, keys = bufs[dbg[0]]
            DMA("pool", "out", dbg_d[:, 0:dbg[1]], ap2[:, 0:dbg[1]], r=keys, w=[("outd",)])
        S.add("pool", None, r=[("outd",)], w=[("outd",)])

        with nc.Block() as block:
            S.emit(nc, block, esem)
    return nc


def _host_consts(inputs, half):
    cf = np.zeros((128, NCF), np.float32)
    C = _cf_off
    p = np.arange(128)
    cf[:, C["ident"]:C["ident"] + 128] = np.eye(128, dtype=np.float32)
    cf[:, C["ones"]:C["ones"] + 128] = 1.0
    cf[:, C["eps"]] = EPS

    def fm(v, n):
        return np.asarray(v, np.float32).reshape(n, 128).T

    cf[:, C["g1"]:C["g1"] + 8] = fm(inputs["ln1_g"][0], 8)
    cf[:, C["b1"]:C["b1"] + 8] = fm(inputs["ln1_b"][0], 8)
    cf[:, C["g2"]:C["g2"] + 8] = fm(inputs["ln2_g"][0], 8)
    cf[:, C["b2"]:C["b2"] + 8] = fm(inputs["ln2_b"][0], 8)
    b_in = np.asarray(inputs["b_in"][0], np.float32)
    cols = []
    for j in range(3):
        cols.append(np.concatenate([b_in[64 * j:64 * j + 64], b_in[64 * (3 + j):64 * (3 + j) + 64]]))
    cols.append(b_in[384:512])
    for li in range(3):
        cols.append(b_in[640 + li * 128:640 + (li + 1) * 128])
    cols.append(b_in[1024:1152])
    cols.append(b_in[1152:1280])
    cols.append(b_in[1280:1408])
    cols.append(b_in[1792:1920])
    cols.append(b_in[1920:2048])
    for rr in range(3):
        for c in range(8):
            cols.append(b_in[2048 + rr * 1024 + c * 128:2048 + rr * 1024 + (c + 1) * 128])
    cf[:, C["binfm"]:C["binfm"] + 36] = np.stack(cols, axis=1)
    bmk = np.asarray(inputs["b_mem_kv"][0], np.float32)
    cf[:, C["bmkfm"]:C["bmkfm"] + 2] = fm(bmk[0:256], 2)
    cf[:, C["sinks"]:C["sinks"] + 6] = np.asarray(inputs["attn_sinks"][0], np.float32)[None, :]
    sl = np.array(SLOPES, np.float64)
    cf[:, C["swacur"]:C["swacur"] + 6] = (sl[None, :] * (p[:, None] - 127)).astype(np.float32)
    cf[:, C["swaprev"]:C["swaprev"] + 6] = (sl[None, :] * (p[:, None] - 127) - 128.0 * sl[None, :]).astype(np.float32)
    cf[:, C["sinkc"]:C["sinkc"] + 6] = (sl[None, :] * (p[:, None] - 127)).astype(np.float32)
    cf[:, C["mb0"]:C["mb0"] + 6] = (sl[None, :] * (p[:, None] - 255)).astype(np.float32)
    cf[:, C["mb1"]:C["mb1"] + 6] = (sl[None, :] * (p[:, None] - 127)).astype(np.float32)
    tg = np.zeros((2, 2, 6, 46), np.float64)
    for bq in range(2):
        for par in range(2):
            for h in range(6):
                for u in range(46):
                    m = max(30 + bq - u, 0)
                    ex = -256.0 * sl[h] * m + (128.0 * sl[h] if par == 0 else 0.0)
                    tg[bq, par, h, u] = np.exp(min(ex, 80.0))
    cf[:, C["tg"]:C["tg"] + tg.size] = tg.reshape(1, -1).astype(np.float32)
    vb = np.full((16, 32), -1e30, np.float32)
    for i in range(16):
        if half == 1:
            vb[i, 0:16] = 0.0
        vb[i, 16:16 + i] = 0.0
    cf[:, C["vbias"]:C["vbias"] + 512] = vb.reshape(1, -1)
    cf[:, C["cmask"]] = 0.0 if half == 1 else -300.0
    cf[:, C["cmask"] + 1] = 1.0 if half == 1 else 0.0
    cf[:, C["binv"]:C["binv"] + 512] = np.concatenate([b_in[512:640], b_in[1408:1792]])[None, :]
    cf[:, C["bmkv"]:C["bmkv"] + 256] = bmk[256:512][None, :]
    cf[:, C["g3"]:C["g3"] + 1024] = np.asarray(inputs["ln3_g"][0], np.float32)[None, :]
    cf[:, C["b3"]:C["b3"] + 1024] = np.asarray(inputs["ln3_b"][0], np.float32)[None, :]
    return cf


def _host_cb():
    cb = np.zeros((128, NCB), np.float32)
    k = np.arange(128)[:, None]
    q = np.arange(128)[None, :]
    cb[:, 0:128] = np.eye(128)
    cb[:, 128:256] = np.where(q >= k, 0.0, -30000.0)
    cb[:, 256:384] = np.where(q < k, 0.0, -30000.0)
    return cb.astype(ml_dtypes.bfloat16)


_NC_CACHE = {}


def kernel(**inputs):
    inputs = {k: np.asarray(v) for k, v in inputs.items()}
    x = inputs["x"].astype(np.float32, copy=False)
    memv = inputs["mem"].astype(np.float32, copy=False)
    if "nc" not in _NC_CACHE:
        _NC_CACHE["nc"] = build_nc()
    nc = _NC_CACHE["nc"]
    cb = _host_cb()
    shared = {
        "wg1": inputs["ffn1_w_gate"][0], "wu1": inputs["ffn1_w_up"][0], "wd1": inputs["ffn1_w_down"][0],
        "wg2": inputs["ffn2_w_gate"][0], "wu2": inputs["ffn2_w_up"][0], "wd2": inputs["ffn2_w_down"][0],
        "w_in": inputs["w_in"][0], "w_mkv": inputs["w_mem_kv"][0], "w_pswa": inputs["w_proj_swa"][0],
        "w_pmoba": inputs["w_proj_moba"][0], "w_pmem": inputs["w_proj_mem"][0], "w_out": inputs["w_out"][0],
        "cb": cb,
    }
    shared = {k: np.ascontiguousarray(v) for k, v in shared.items()}
    in_maps = []
    for core in range(8):
        b, half = core // 2, core % 2
        m = dict(shared)
        m["x_main"] = np.ascontiguousarray(x[b, half * 4096:(half + 1) * 4096])
        m["x_other"] = np.ascontiguousarray(x[b, 0:4096])
        m["mem"] = np.ascontiguousarray(memv[b])
        m["cf"] = _host_consts(inputs, half)
        in_maps.append(m)
    res = run_bass_kernel_spmd(nc, in_maps, core_ids=list(range(8)))
    outp = np.empty((4, 8192, 1024), np.float32)
    for core in range(8):
        b, half = core // 2, core % 2
        outp[b, half * 4096:(half + 1) * 4096] = np.asarray(res.results[core]["out"]).reshape(4096, 1024)
    return outp
```
